# Optimizing a Trainium2 kernel written in Bass

```python
import jax, jax.numpy as jnp
from jax import lax
import numpy as np

D_MODEL = 1024
BATCH = 16
SEQ = 2048
DEPTH = 1

W_RWKV = D_MODEL // 2
HD_RWKV = 64
H_RWKV = W_RWKV // HD_RWKV
LORA_DECAY = 32
LORA_ICLR = 32
LORA_GATE = 96
GN_EPS = 64e-5

W_GLA = D_MODEL - W_RWKV
DV_GLA = 128
H_GLA = W_GLA // DV_GLA
DK_GLA = DV_GLA // 2
LORA_GK = 16
GK_NORMALIZER = 16.0
GLA_CHUNK = 64
GLA_EPS = 1e-5

D_FF = ((8 * D_MODEL // 3 + 255) // 256) * 256
RMS_EPS = 1e-6

RWKV_SPLITS = (W_RWKV, W_RWKV, W_RWKV, LORA_DECAY, LORA_ICLR, LORA_GATE)
GLA_SPLITS = (H_GLA * DK_GLA, H_GLA * DK_GLA, W_GLA, W_GLA, LORA_GK)
D_RWKV_IN = sum(RWKV_SPLITS)
D_GLA_IN = sum(GLA_SPLITS)
D_IN = D_RWKV_IN + D_GLA_IN

kernel_name = "hymba_rwkv7_gla_swiglu"


def _split(z, sizes):
    idx = [int(i) for i in np.cumsum(sizes)[:-1]]
    return jnp.split(z, idx, axis=-1)


def rms_norm(x, g, eps=RMS_EPS):
    xf = x.astype(jnp.float32)
    y = xf * lax.rsqrt(jnp.mean(xf * xf, axis=-1, keepdims=True) + eps)
    return (y * g.astype(jnp.float32)).astype(x.dtype)


def token_shift(z):
    return jnp.pad(z[:, :-1], ((0, 0), (1, 0), (0, 0)))


def rwkv7_mixer(z, mu, decay_base, decay_up, iclr_base, iclr_up, gate_up,
                k_k, k_a, r_k, lnx_w, lnx_b):
    B, T, _ = z.shape
    z = z.astype(jnp.float32)
    z = z + (token_shift(z) - z) * mu
    r, k, v, w_lo, a_lo, g_lo = _split(z, RWKV_SPLITS)
    log_w = -jax.nn.softplus(-(decay_base + jnp.tanh(w_lo) @ decay_up)) - 0.5
    decay = jnp.exp(-jnp.exp(log_w))
    a = jax.nn.sigmoid(iclr_base + a_lo @ iclr_up)
    g = jax.nn.sigmoid(g_lo) @ gate_up

    def heads(t):
        return t.reshape(B, T, H_RWKV, HD_RWKV)

    kk = heads(k * k_k)
    kk = kk * lax.rsqrt(jnp.maximum(jnp.sum(kk * kk, axis=-1, keepdims=True), 1e-24))
    k = k * (1.0 + (a - 1.0) * k_a)
    r_h, k_h, v_h, w_h, a_h = heads(r), heads(k), heads(v), heads(decay), heads(a)
    rem = -kk
    add = kk * a_h

    def step(S, inp):
        r_t, w_t, k_t, v_t, a_t, b_t = inp
        sa = jnp.einsum('bhvk,bhk->bhv', S, a_t)
        S = (S * w_t[:, :, None, :] + sa[..., None] * b_t[:, :, None, :]
             + v_t[..., None] * k_t[:, :, None, :])
        y = jnp.einsum('bhvk,bhk->bhv', S, r_t)
        return S, y

    S0 = jnp.zeros((B, H_RWKV, HD_RWKV, HD_RWKV), jnp.float32)
    xs = (jnp.moveaxis(r_h, 1, 0), jnp.moveaxis(w_h, 1, 0), jnp.moveaxis(k_h, 1, 0),
          jnp.moveaxis(v_h, 1, 0), jnp.moveaxis(rem, 1, 0), jnp.moveaxis(add, 1, 0))
    _, y = lax.scan(step, S0, xs)
    y = jnp.moveaxis(y, 0, 1)
    mean = jnp.mean(y, axis=-1, keepdims=True)
    var = jnp.mean(jnp.square(y - mean), axis=-1, keepdims=True)
    y = ((y - mean) * lax.rsqrt(var + GN_EPS)).reshape(B, T, W_RWKV) * lnx_w + lnx_b
    bonus = jnp.sum(r_h * k_h * r_k, axis=-1, keepdims=True) * v_h
    return (y + bonus.reshape(B, T, W_RWKV)) * g


def gla_mixer(z, gk_up, gk_bias, norm_g):
    B, T, _ = z.shape
    C = GLA_CHUNK
    N = T // C
    z = z.astype(jnp.float32)
    q, k, v, og, gk_lo = _split(z, GLA_SPLITS)
    gk = jax.nn.log_sigmoid(gk_lo @ gk_up + gk_bias) / GK_NORMALIZER

    def chunks(t, d):
        return t.reshape(B, N, C, H_GLA, d).transpose(0, 3, 1, 2, 4)

    q = chunks(q, DK_GLA) * (DK_GLA ** -0.5)
    k = chunks(k, DK_GLA)
    v = chunks(v, DV_GLA)
    b = jnp.cumsum(chunks(gk, DK_GLA), axis=3)
    b_last = b[:, :, :, -1:, :]
    q_in = q * jnp.exp(b)
    k_in = k * jnp.exp(-b)
    k_end = k * jnp.exp(b_last - b)

    causal = jnp.tril(jnp.ones((C, C), dtype=bool))
    att = jnp.where(causal, jnp.einsum('bhncd,bhnsd->bhncs', q_in, k_in), 0.0)
    o = jnp.einsum('bhncs,bhnsv->bhncv', att, v)

    delta = jnp.einsum('bhncd,bhncv->bhndv', k_end, v)
    chunk_decay = jnp.exp(b_last[:, :, :, 0, :])

    def carry_step(S, inp):
        dec, dS = inp
        return S * dec[..., None] + dS, S

    S0 = jnp.zeros((B, H_GLA, DK_GLA, DV_GLA), jnp.float32)
    _, S_prev = lax.scan(carry_step, S0, (jnp.moveaxis(chunk_decay, 2, 0), jnp.moveaxis(delta, 2, 0)))
    S_prev = jnp.moveaxis(S_prev, 0, 2)
    o = o + jnp.einsum('bhncd,bhndv->bhncv', q_in, S_prev)

    o = o.transpose(0, 2, 3, 1, 4).reshape(B, T, H_GLA, DV_GLA)
    o = o * lax.rsqrt(jnp.mean(o * o, axis=-1, keepdims=True) + GLA_EPS) * norm_g
    return o.reshape(B, T, W_GLA) * jax.nn.silu(og)


def setup_inputs(seed: int = 0) -> dict:
    key = jax.random.key(seed)
    ks = jax.random.split(key, 24)
    L, D = DEPTH, D_MODEL
    nrm = jax.random.normal
    f32 = jnp.float32
    return {
        "x": nrm(ks[0], (BATCH, SEQ, D), f32),
        "rms1_g": 1.0 + 0.02 * nrm(ks[1], (L, D), f32),
        "w_in": nrm(ks[2], (L, D, D_IN), f32) * D ** -0.5,
        "mu_shift": jax.random.uniform(ks[3], (L, D_RWKV_IN), f32),
        "decay_base": jax.random.uniform(ks[4], (L, W_RWKV), f32, -5.0, 0.0),
        "decay_up": 0.1 * nrm(ks[5], (L, LORA_DECAY, W_RWKV), f32),
        "iclr_base": 0.5 * nrm(ks[6], (L, W_RWKV), f32),
        "iclr_up": 0.5 * nrm(ks[7], (L, LORA_ICLR, W_RWKV), f32) * LORA_ICLR ** -0.5,
        "gate_up": nrm(ks[8], (L, LORA_GATE, W_RWKV), f32) * LORA_GATE ** -0.5,
        "k_k": 0.85 + 0.05 * nrm(ks[9], (L, W_RWKV), f32),
        "k_a": 1.0 + 0.05 * nrm(ks[10], (L, W_RWKV), f32),
        "r_k": 0.1 * nrm(ks[11], (L, H_RWKV, HD_RWKV), f32),
        "lnx_w": 1.0 + 0.02 * nrm(ks[12], (L, W_RWKV), f32),
        "lnx_b": 0.02 * nrm(ks[13], (L, W_RWKV), f32),
        "gk_up": nrm(ks[14], (L, LORA_GK, H_GLA * DK_GLA), f32) * LORA_GK ** -0.5,
        "gk_bias": 0.1 * nrm(ks[15], (L, H_GLA * DK_GLA), f32),
        "gla_norm_g": 1.0 + 0.02 * nrm(ks[16], (L, DV_GLA), f32),
        "w_out": nrm(ks[17], (L, D, D), f32) * D ** -0.5,
        "rms2_g": 1.0 + 0.02 * nrm(ks[18], (L, D), f32),
        "ffn_gate": nrm(ks[19], (L, D, D_FF), f32) * D ** -0.5,
        "ffn_up": nrm(ks[20], (L, D, D_FF), f32) * D ** -0.5,
        "ffn_down": nrm(ks[21], (L, D_FF, D), f32) * D_FF ** -0.5,
        "final_g": 1.0 + 0.02 * nrm(ks[22], (D,), f32),
    }


def reference(x, rms1_g, w_in, mu_shift, decay_base, decay_up, iclr_base, iclr_up,
              gate_up, k_k, k_a, r_k, lnx_w, lnx_b, gk_up, gk_bias, gla_norm_g,
              w_out, rms2_g, ffn_gate, ffn_up, ffn_down, final_g):
    h = x
    for l in range(DEPTH):
        n = rms_norm(h, rms1_g[l])
        z = n @ w_in[l]
        y_rwkv = rwkv7_mixer(z[..., :D_RWKV_IN], mu_shift[l], decay_base[l], decay_up[l],
                             iclr_base[l], iclr_up[l], gate_up[l], k_k[l], k_a[l],
                             r_k[l], lnx_w[l], lnx_b[l])
        y_gla = gla_mixer(z[..., D_RWKV_IN:], gk_up[l], gk_bias[l], gla_norm_g[l])
        mix = jnp.concatenate([y_rwkv, y_gla], axis=-1).astype(h.dtype)
        h = h + mix @ w_out[l]
        n = rms_norm(h, rms2_g[l])
        h = h + (jax.nn.silu(n @ ffn_gate[l]) * (n @ ffn_up[l])) @ ffn_down[l]
    return rms_norm(h, final_g)
```

```python
import math
from contextlib import ExitStack

import numpy as np
import concourse.bass as bass
import concourse.mybir as mybir
from concourse.bass_utils import run_bass_kernel_spmd

F32 = mybir.dt.float32
BF16 = mybir.dt.bfloat16
ALU = mybir.AluOpType
AF = mybir.ActivationFunctionType
AX = mybir.AxisListType

D = 1024
DIN = 3248
DRW = 1696
DFF = 2816
NFF = DFF // 128
CDEC = -math.exp(-0.5)
GN_EPS = 64e-5
GLA_EPS = 1e-5
RMS_EPS = 1e-6


class Builder:
    def __init__(self, nc, es):
        self.nc = nc
        self.es = es
        self.eng = {"pe": nc.tensor, "dve": nc.vector, "act": nc.scalar, "pool": nc.gpsimd, "sp": nc.sync}
        self.semh = {}
        self.cnt = {}
        for n in self.eng:
            self.semh[n] = es.enter_context(nc.semaphore("prog_" + n))
            self.cnt[n] = 0
        self.waited = {}
        self.lw = {}
        self.rd = {}
        self.ndma = 0
        self.last_pe = None
        self.last_pe_inc = True
        self.last_rg = "F"

    def sb(self, name, shape, dt):
        return self.es.enter_context(self.nc.sbuf_tensor(name, list(shape), dt))

    def ps(self, name, shape, dt):
        return self.es.enter_context(self.nc.psum_tensor(name, list(shape), dt))

    def _deps(self, reads, writes):
        deps = {}
        for k in reads:
            w = self.lw.get(k)
            if w:
                deps[w[0]] = max(deps.get(w[0], 0), w[1])
        for k in writes:
            w = self.lw.get(k)
            if w:
                deps[w[0]] = max(deps.get(w[0], 0), w[1])
            for s, v in self.rd.get(k, {}).items():
                deps[s] = max(deps.get(s, 0), v)
        return deps

    def _wait(self, eng, deps):
        for s, v in deps.items():
            if eng == "pe" and s == "pe":
                continue
            if self.waited.get((eng, s), 0) >= v:
                continue
            self.eng[eng].wait_ge(self.semh[s], v)
            self.waited[(eng, s)] = v

    def _record(self, reads, writes, tag):
        s, v = tag
        for k in reads:
            d = self.rd.setdefault(k, {})
            d[s] = max(d.get(s, 0), v)
        for k in writes:
            self.lw[k] = tag
            self.rd[k] = {}

    def op(self, eng, reads, writes, fn, inc=True, rg="F"):
        if eng == "pe" and rg != self.last_rg and self.last_pe is not None:
            if not self.last_pe_inc:
                self.last_pe.then_inc(self.semh["pe"], 1)
                self.cnt["pe"] += 1
                self.last_pe_inc = True
            if self.waited.get(("pe", "pe"), 0) < self.cnt["pe"]:
                self.eng["pe"].wait_ge(self.semh["pe"], self.cnt["pe"])
                self.waited[("pe", "pe")] = self.cnt["pe"]
        self._wait(eng, self._deps(reads, writes))
        ins = fn(self.eng[eng])
        if eng == "pe":
            self.last_pe, self.last_pe_inc, self.last_rg = ins, inc, rg
        if inc:
            ins.then_inc(self.semh[eng], 1)
            self.cnt[eng] += 1
            v = self.cnt[eng]
        else:
            v = self.cnt[eng] + 1
        self._record(reads, writes, (eng, v))

    def dma(self, q, out, in_, reads, writes, key):
        self._wait(q, self._deps(reads, writes))
        sname = "dma_" + key
        if sname not in self.semh:
            self.semh[sname] = self.es.enter_context(self.nc.semaphore(sname))
            self.cnt[sname] = 0
        self.eng[q].dma_start(out=out, in_=in_).then_inc(self.semh[sname], 16)
        self.cnt[sname] += 16
        self._record(reads, writes, (sname, self.cnt[sname]))

    def group_done(self, key, keys):
        sname = "dma_" + key
        for k in keys:
            self.lw[k] = (sname, self.cnt[sname])

    def barrier(self):
        for e in self.eng:
            for s, v in self.cnt.items():
                if s == e and e in ("pe", "sp"):
                    continue
                if v > 0 and self.waited.get((e, s), 0) < v:
                    self.eng[e].wait_ge(self.semh[s], v)
                    self.waited[(e, s)] = v


class _Stop(Exception):
    pass


def build_program(T, stop=None):
    nc = bass.Bass("TRN2", target_bir_lowering=False)
    try:
        _build(nc, T, stop)
    except _Stop:
        pass
    return nc


def _build(nc, T, stop):
    NU = T // 64
    TOK2 = min(256, T)
    NS2 = TOK2 // 128
    NT2 = (2 * T) // TOK2
    def din(name, shape, dt=F32):
        return nc.dram_tensor(name, list(shape), dt, kind="ExternalInput").ap()

    x = din("x", [2, T, D])
    w_in = din("w_in", [D, DIN])
    w_out = din("w_out", [D, D])
    ffn_gate = din("ffn_gate", [D, DFF])
    ffn_up = din("ffn_up", [D, DFF])
    ffn_down = din("ffn_down", [DFF, D])
    mu_row = din("mu_row", [1, DRW])
    gcols = din("gcols", [128, 24])
    decay_up = din("decay_up", [32, 512])
    iclr_up = din("iclr_up", [32, 512])
    gate_up = din("gate_up", [96, 512])
    gk_up = din("gk_up", [16, 256])
    bias_row = din("bias_row", [1, 1280])
    cb_row = din("cb_row", [1, 2688])
    fg_row = din("fg_row", [1, D])
    cident = din("cident", [128, 128])
    cmask = din("cmask", [128, 640])
    cind = din("cind", [128, 2])
    out = nc.dram_tensor("out", [2, T, D], F32, kind="ExternalOutput").ap()
    mixscr = nc.dram_tensor("mixscr", [2, T, D], BF16, kind="ExternalOutput").ap()

    with ExitStack() as es0:
        B = Builder(nc, es0)
        op, dma = B.op, B.dma

        def CP(n):
            if stop == n:
                B.barrier()
                raise _Stop()

        zb = [B.ps("zb%d" % i, [128, 512], F32) for i in range(2)]
        lb = B.ps("lb", [128, 512], F32)
        tpb = B.ps("tpb", [128, 8, 128], BF16)
        gb = [B.ps("gb%d" % i, [128, 512], F32) for i in range(3)]
        sb7 = B.ps("sb7", [128, 512], F32)
        gstate = {"i": 0}

        def G():
            i = gstate["i"] % 3
            gstate["i"] += 1
            return gb[i], "gb%d" % i

        identf = B.sb("identf", [128, 128], F32)
        identb = B.sb("identb", [128, 128], BF16)
        epsc = B.sb("epsc", [128, 4], F32)
        op("pool", [], ["epsc"], lambda e: e.memset(epsc[:, 0:1], RMS_EPS))
        op("pool", ["epsc"], ["epsc"], lambda e: e.memset(epsc[:, 1:2], GN_EPS))
        op("pool", ["epsc"], ["epsc"], lambda e: e.memset(epsc[:, 2:3], GLA_EPS))
        dma("sp", identf[:], cident[:, :], [], ["identf"], "c0")

        with ExitStack() as es1:
            B.es = es1
            Wa = B.sb("Wa", [128, 8, DIN], BF16)
            Wb = B.sb("Wb", [128, 8, DRW], BF16)
            g1c = B.sb("gcol", [128, 24], F32)
            maskt = B.sb("maskt", [128, 640], F32)
            shb = B.sb("shb", [128, 256], BF16)
            indt = B.sb("indt", [128, 2], F32)
            maskb = B.sb("maskb", [128, 384], BF16)
            indb = B.sb("indb", [128, 2], BF16)
            CB = B.sb("CB", [128, 2688], F32)
            DU = B.sb("DU", [32, 512], BF16)
            IU = B.sb("IU", [32, 512], BF16)
            GU = B.sb("GU", [96, 512], BF16)
            GKU = B.sb("GKU", [16, 256], BF16)
            BH = B.sb("BH", [1, 1280], BF16)
            BL = B.sb("BL", [1, 1280], BF16)
            ones1 = B.sb("ones1", [1, 128], BF16)

            dma("sp", g1c[:], gcols[:, :], [], ["gcol"], "c0")
            dma("sp", maskt[:], cmask[:, :], [], ["maskt"], "c0")
            dma("sp", indt[:], cind[:, :], [], ["indt"], "c0")
            dma("sp", CB[:], cb_row[0:1, :].partition_broadcast(128), [], ["CB"], "c0")
            B.group_done("c0", ["identf", "gcol", "maskt", "indt", "CB"])
            op("dve", ["identf"], ["identb"], lambda e: e.tensor_copy(out=identb[:], in_=identf[:]))
            op("dve", ["maskt"], ["shb"], lambda e: e.tensor_copy(out=shb[:], in_=maskt[:, 384:640]))
            op("dve", ["maskt"], ["maskb"], lambda e: e.tensor_copy(out=maskb[:], in_=maskt[:, 0:384]))
            op("dve", ["indt"], ["indb"], lambda e: e.tensor_copy(out=indb[:], in_=indt[:]))
            op("pool", [], ["ones1"], lambda e: e.memset(ones1[:], 1.0))

            with ExitStack() as es_s:
                B.es = es_s
                stg = B.sb("stg", [128, 1280], F32)
                stg2 = B.sb("stg2", [128, 1280], F32)
                mub = B.sb("mub", [128, DRW], F32)
                omm = B.sb("omm", [128, DRW], F32)
                wst = [B.sb("wst%d" % i, [128, DIN], F32) for i in range(2)]
                dma("sp", mub[:], mu_row[0:1, :].partition_broadcast(128), [], ["mub"], "mub")
                dma("sp", stg[0:32, 0:512], decay_up[:, :], [], ["stg_a"], "stg_a")
                dma("sp", stg[0:32, 512:1024], iclr_up[:, :], [], ["stg_b"], "stg_b")
                dma("sp", stg2[0:96, 0:512], gate_up[:, :], [], ["stg2_a"], "stg2_a")
                dma("sp", stg2[0:16, 512:768], gk_up[:, :], [], ["stg2_b"], "stg2_b")
                dma("sp", stg[96:97, 0:1280], bias_row[0:1, :], [], ["stg_c"], "stg_c")
                op("dve", ["mub"], ["omm"], lambda e: e.tensor_scalar(
                    out=omm[:], in0=mub[:], scalar1=-1.0, scalar2=1.0, op0=ALU.mult, op1=ALU.add))
                op("dve", ["stg_a"], ["DU"], lambda e: e.tensor_copy(out=DU[:], in_=stg[0:32, 0:512]))
                op("dve", ["stg_b"], ["IU"], lambda e: e.tensor_copy(out=IU[:], in_=stg[0:32, 512:1024]))
                op("dve", ["stg2_a"], ["GU"], lambda e: e.tensor_copy(out=GU[:], in_=stg2[0:96, 0:512]))
                op("dve", ["stg2_b"], ["GKU"], lambda e: e.tensor_copy(out=GKU[:], in_=stg2[0:16, 512:768]))
                brow = B.sb("brow", [1, 1280], F32)
                bhi32 = B.sb("bhi32", [1, 1280], F32)
                dma("sp", brow[:], bias_row[0:1, :], [], ["brow"], "brow")
                op("dve", ["brow"], ["BH"], lambda e: e.tensor_copy(out=BH[:], in_=brow[:]))
                op("dve", ["BH"], ["bhi32"], lambda e: e.tensor_copy(out=bhi32[:], in_=BH[:]))
                op("dve", ["brow", "bhi32"], ["bhi32b"], lambda e: e.tensor_tensor(
                    out=bhi32[:], in0=brow[:], in1=bhi32[:], op=ALU.subtract))
                op("dve", ["bhi32b"], ["BL"], lambda e: e.tensor_copy(out=BL[:], in_=bhi32[:]))
                for k in range(8):
                    ws = wst[k % 2]
                    wk = "wst%d" % (k % 2)
                    dma("sp", ws[:], w_in[k * 128:(k + 1) * 128, :], [], [wk], wk)
                    op("dve", [wk, "omm", "gcol"], ["Wa"], lambda e, ws=ws, k=k: e.scalar_tensor_tensor(
                        out=Wa[:, k, 0:DRW], in0=ws[:, 0:DRW], scalar=g1c[:, k:k + 1], in1=omm[:],
                        op0=ALU.mult, op1=ALU.mult))
                    op("dve", [wk, "mub", "gcol"], ["Wb"], lambda e, ws=ws, k=k: e.scalar_tensor_tensor(
                        out=Wb[:, k, :], in0=ws[:, 0:DRW], scalar=g1c[:, k:k + 1], in1=mub[:],
                        op0=ALU.mult, op1=ALU.mult))
                    op("act", [wk, "gcol"], ["Wa2"], lambda e, ws=ws, k=k: e.activation(
                        out=Wa[:, k, DRW:DIN], in_=ws[:, DRW:DIN], func=AF.Copy, scale=g1c[:, k:k + 1]))
                B.barrier()
            B.es = es1
            CP(1)

            xt = [B.sb("xt%d" % i, [128, D], F32) for i in range(2)]
            sm = B.sb("sm", [128, 64], F32)
            xnb = [B.sb("xn%d" % i, [128, D], BF16) for i in range(2)]
            xT = B.sb("xT", [128, 8, 128], BF16)
            xTs = B.sb("xTs", [128, 8, 128], BF16)
            rz = B.sb("rz", [128, 512], F32)
            kz = B.sb("kz", [128, 512], F32)
            vb = B.sb("vb", [128, 512], BF16)
            qkz = B.sb("qkz", [128, 512], F32)
            gvb = B.sb("gvb", [128, 512], BF16)
            osl = B.sb("osl", [128, 512], F32)
            TW = B.sb("TW", [32, 128], BF16)
            TA = B.sb("TA", [32, 128], BF16)
            TS = B.sb("TS", [96, 128], BF16)
            TG = B.sb("TG", [16, 128], BF16)
            sg = B.sb("sg", [128, 512], F32)
            t1 = sg
            av = B.sb("av", [128, 512], F32)
            gz = B.sb("gz", [128, 512], BF16)
            lsp = B.sb("lsp", [128, 256], F32)
            sgh = B.sb("sgh", [128, 512], BF16)
            sgl = B.sb("sgl", [128, 512], BF16)
            lsh = B.sb("lsh", [128, 256], BF16)
            lsl = B.sb("lsl", [128, 256], BF16)
            E0 = B.sb("E0", [128, 512], F32)
            E1 = B.sb("E1", [128, 512], F32)
            ysq, yn = E0, E1
            Eg = B.sb("Eg", [128, 3, 256], F32)
            WC = B.sb("WC", [128, 4, 2], F32)
            DEC = B.sb("DEC", [128, 2, 2], F32)
            kk = B.sb("kk", [128, 512], F32)
            k2 = B.sb("k2", [128, 512], F32)
            ew = k2
            bv = B.sb("bv", [128, 512], F32)
            LT = bv[:].rearrange("p (a t) -> p a t", a=4)
            Rt = B.sb("Rt", [128, 512], BF16)
            At = B.sb("At", [128, 512], BF16)
            XA = B.sb("XA", [128, 8, 2, 64], BF16)
            Bt = B.sb("Bt", [128, 512], BF16)
            Kt = B.sb("Kt", [128, 512], BF16)
            Be = B.sb("Be", [128, 512], BF16)
            Ke = B.sb("Ke", [128, 512], BF16)
            ART = B.sb("ART", [128, 4, 2, 128], BF16)
            BKT = B.sb("BKT", [128, 2, 4, 128], BF16)
            RT0 = B.sb("RT0", [128, 4, 128], BF16)
            RT1 = B.sb("RT1", [128, 4, 128], BF16)
            AT0 = B.sb("AT0", [128, 4, 128], BF16)
            AT1 = B.sb("AT1", [128, 4, 128], BF16)
            QL = B.sb("QL", [128, 8, 2, 128], BF16)
            AL = B.sb("AL", [128, 8, 2, 128], BF16)
            Qg = [B.sb("Qg%d" % i, [128, 4, 128], BF16) for i in range(2)]
            QTg = [B.sb("QTg%d" % i, [128, 4, 128], BF16) for i in range(2)]
            PTg = [B.sb("PTg%d" % i, [128, 4, 128], BF16) for i in range(2)]
            PTf = B.sb("PTf", [128, 8, 128], BF16)
            UL32 = B.sb("UL32", [128, 8, 64], F32)
            Ahb = B.sb("Ahb", [128, 8, 64], BF16)
            Ub = B.sb("Ub", [128, 8, 64], BF16)
            M32 = B.sb("M32", [128, 4, 2, 64], F32)
            Mb = B.sb("Mb", [128, 4, 2, 64], BF16)
            tmp2k = B.sb("tmp2k", [128, 512], F32)
            mtmp = tmp2k[:].rearrange("p (a b d) -> p a b d", a=4, b=2)
            stmp = tmp2k[:].rearrange("p (a b d) -> p a b d", a=2, b=2)
            MIX = B.sb("MIX", [128, D], BF16)
            QI = B.sb("QI", [128, 256], BF16)
            KI = B.sb("KI", [128, 256], BF16)
            KE = B.sb("KE", [128, 256], BF16)
            GT = B.sb("GT", [128, 4, 128], BF16)
            QG0 = B.sb("QG0", [128, 2, 128], BF16)
            QG1 = B.sb("QG1", [128, 2, 128], BF16)
            attT = B.sb("attT", [128, 4, 128], BF16)
            S32 = B.sb("S32", [128, 2, 2, 128], F32)
            Sb = B.sb("Sb", [128, 2, 2, 128], BF16)

            for t_, nm in ((xnb[1], "xn1"), (RT0, "RT0"), (RT1, "RT1"), (AT0, "AT0"), (AT1, "AT1"),
                           (QG0, "QG0"), (QG1, "QG1"), (M32, "M32"), (Mb, "Mb"), (S32, "S32"), (Sb, "Sb")):
                op("pool", [], [nm], lambda e, t_=t_: e.memset(t_[:], 0.0))

            m_su = maskt[:, 0:128]
            m_iu = maskt[:, 128:256]
            m_sl = maskt[:, 256:384]
            mb_su = maskb[:, 0:128]
            mb_iu = maskb[:, 128:256]
            mb_sl = maskb[:, 256:384]
            MK1 = maskt[:, 0:256].unsqueeze(1).to_broadcast([128, 2, 256])
            MK3 = m_sl.unsqueeze(1).to_broadcast([128, 4, 128])
            MKiu = m_iu.unsqueeze(1).to_broadcast([128, 4, 128])
            CBkk, CBka, CBrk = CB[:, 0:512], CB[:, 512:1024], CB[:, 1024:1536]
            CBlw, CBlb = CB[:, 1536:2048], CB[:, 2048:2560]
            CBgn = CB[:, 2560:2688].unsqueeze(1).to_broadcast([128, 4, 128])

            def load_x(i):
                s = i % 2
                dma("sp", xt[s][0:64, :], x[0, i * 64:(i + 1) * 64, :], [], ["xt%da" % s], "xt%da" % s)
                dma("sp", xt[s][64:128, :], x[1, i * 64:(i + 1) * 64, :], [], ["xt%db" % s], "xt%db" % s)

            def h3(ap, n):
                return ap.rearrange("p (h d) -> p h d", h=n)

            def bc(ap2, n):
                return ap2.unsqueeze(2).to_broadcast([128, ap2.shape[1], n])

            load_x(0)
            for i in range(NU):
                s = i % 2
                if i + 1 < NU:
                    load_x(i + 1)
                xk = ["xt%da" % s, "xt%db" % s]
                xn, xnk = xnb[s], "xn%d" % s
                xnp, xnpk = xnb[1 - s], "xn%d" % (1 - s)
                op("act", xk, [xnk, "ss"], lambda e: e.activation(
                    out=xn[:], in_=xt[s][:], func=AF.Square, accum_out=sm[:, 0:1]))
                op("act", ["ss"], ["ms"], lambda e: e.activation(
                    out=sm[:, 1:2], in_=sm[:, 0:1], func=AF.Ln, scale=1.0 / D, bias=epsc[:, 0:1]))
                op("act", ["ms"], ["rstd"], lambda e: e.activation(
                    out=sm[:, 2:3], in_=sm[:, 1:2], func=AF.Exp, scale=-0.5))
                op("act", xk + ["rstd"], [xnk], lambda e: e.activation(
                    out=xn[:], in_=xt[s][:], func=AF.Copy, scale=sm[:, 2:3]))
                for k in range(8):
                    op("pe", [xnk, "identb"], ["tpb"], lambda e, k=k: e.transpose(
                        out=tpb[:, k, :], in_=xn[:, k * 128:(k + 1) * 128], identity=identb[:]), inc=(k == 7))
                op("act", ["tpb"], ["xT"], lambda e: e.activation(out=xT[:], in_=tpb[:], func=AF.Copy))
                for hf in range(2):
                    for kk_ in range(4):
                        k = 4 * hf + kk_
                        o_ = zb[hf][:, kk_ * 128:(kk_ + 1) * 128]
                        op("pe", [xnk, "shb"], ["zb%d" % hf], lambda e, k=k, o_=o_: e.matmul(
                            o_, lhsT=xn[:, k * 128:(k + 1) * 128], rhs=shb[:, 0:128], start=True, stop=False), inc=False)
                        op("pe", [xnpk, "shb"], ["zb%d" % hf], lambda e, k=k, o_=o_: e.matmul(
                            o_, lhsT=xnp[:, k * 128:(k + 1) * 128], rhs=shb[:, 128:256], start=False, stop=True), inc=(kk_ == 3))
                op("act", ["zb0"], ["xTs_m"], lambda e: e.activation(
                    out=xTs[:, 0:4, :].rearrange("p k t -> p (k t)"), in_=zb[0][:], func=AF.Copy))
                op("dve", ["zb1"], ["xTs_c"], lambda e: e.tensor_copy(
                    out=xTs[:, 4:8, :].rearrange("p k t -> p (k t)"), in_=zb[1][:]))
                XS = ["xTs_m", "xTs_c"]
                CP(2)

                def proj(bank, bkey, c0, n, shift):
                    for k in range(8):
                        op("pe", ["xT", "Wa", "Wa2"], [bkey], lambda e, k=k: e.matmul(
                            bank[:, 0:n], lhsT=xT[:, k, :], rhs=Wa[:, k, c0:c0 + n], start=(k == 0),
                            stop=(k == 7 and not shift)), inc=(k == 7 and not shift))
                    if shift:
                        for k in range(8):
                            op("pe", XS + ["Wb"], [bkey], lambda e, k=k: e.matmul(
                                bank[:, 0:n], lhsT=xTs[:, k, :], rhs=Wb[:, k, c0:c0 + n], start=False,
                                stop=(k == 7)), inc=(k == 7))

                proj(zb[0], "zb0", 0, 512, True)
                op("act", ["zb0"], ["rz"], lambda e: e.activation(out=rz[:], in_=zb[0][:], func=AF.Copy))
                proj(zb[1], "zb1", 512, 512, True)
                op("act", ["zb1"], ["kz"], lambda e: e.activation(out=kz[:], in_=zb[1][:], func=AF.Copy))
                proj(zb[0], "zb0", 1024, 512, True)
                op("act", ["zb0"], ["vb"], lambda e: e.activation(out=vb[:], in_=zb[0][:], func=AF.Copy))
                proj(zb[1], "zb1", 1696, 512, False)
                op("act", ["zb1"], ["qkz"], lambda e: e.activation(out=qkz[:], in_=zb[1][:], func=AF.Copy))
                proj(zb[0], "zb0", 2208, 512, False)
                op("act", ["zb0"], ["gvb"], lambda e: e.activation(out=gvb[:], in_=zb[0][:], func=AF.Copy))
                proj(zb[1], "zb1", 2720, 512, False)
                op("act", ["zb1"], ["osl"], lambda e: e.activation(out=osl[:], in_=zb[1][:], func=AF.Sigmoid))
                op("dve", ["osl", "zb1"], ["osl"], lambda e: e.tensor_tensor(out=osl[:], in0=osl[:], in1=zb[1][:], op=ALU.mult))

                def lproj(m, col, c0, shift):
                    for k in range(8):
                        op("pe", ["xT", "Wa", "Wa2"], ["lb"], lambda e, k=k: e.matmul(
                            lb[0:m, col:col + 128], lhsT=Wa[:, k, c0:c0 + m], rhs=xT[:, k, :], start=(k == 0),
                            stop=(k == 7 and not shift)), inc=(k == 7 and not shift))
                    if shift:
                        for k in range(8):
                            op("pe", XS + ["Wb"], ["lb"], lambda e, k=k: e.matmul(
                                lb[0:m, col:col + 128], lhsT=Wb[:, k, c0:c0 + m], rhs=xTs[:, k, :], start=False,
                                stop=(k == 7)), inc=(k == 7))

                lproj(32, 0, 1536, True)
                lproj(32, 384, 1568, True)
                lproj(96, 128, 1600, True)
                lproj(16, 256, 3232, False)
                op("act", ["lb"], ["TW"], lambda e: e.activation(out=TW[:], in_=lb[0:32, 0:128], func=AF.Tanh))
                op("act", ["lb"], ["TA"], lambda e: e.activation(out=TA[:], in_=lb[0:32, 384:512], func=AF.Copy))
                op("act", ["lb"], ["TS"], lambda e: e.activation(out=TS[:], in_=lb[0:96, 128:256], func=AF.Sigmoid))
                op("act", ["lb"], ["TG"], lambda e: e.activation(out=TG[:], in_=lb[0:16, 256:384], func=AF.Copy))

                CP(3)
                def lup(lhsT, rhs, n, boff, keys):
                    bank, bk = G()
                    nb = boff is not None
                    op("pe", keys, [bk], lambda e: e.matmul(bank[:, 0:n], lhsT=lhsT, rhs=rhs, start=True, stop=not nb),
                       inc=not nb)
                    if nb:
                        op("pe", ["ones1", "BH"], [bk], lambda e: e.matmul(
                            bank[:, 0:n], lhsT=ones1[0:1, :], rhs=BH[0:1, boff:boff + n], start=False, stop=False), inc=False)
                        op("pe", ["ones1", "BL"], [bk], lambda e: e.matmul(
                            bank[:, 0:n], lhsT=ones1[0:1, :], rhs=BL[0:1, boff:boff + n], start=False, stop=True))
                    return bank, bk

                bk_w, kw = lup(TW[0:32, :], DU[0:32, :], 512, 0, ["TW", "DU"])
                op("act", [kw], ["sg"], lambda e: e.activation(out=sg[:], in_=bk_w[:], func=AF.Sigmoid))
                op("dve", ["sg"], ["sgh"], lambda e: e.tensor_copy(out=sgh[:], in_=sg[:]))
                op("dve", ["sg", "sgh"], ["sgl"], lambda e: e.tensor_tensor(out=sgl[:], in0=sg[:], in1=sgh[:], op=ALU.subtract))
                bk_a, ka = lup(TA[0:32, :], IU[0:32, :], 512, 512, ["TA", "IU"])
                op("act", [ka], ["av"], lambda e: e.activation(out=av[:], in_=bk_a[:], func=AF.Sigmoid))
                bk_g, kg = lup(TS[0:96, :], GU[0:96, :], 512, None, ["TS", "GU"])
                op("act", [kg], ["gz"], lambda e: e.activation(out=gz[:], in_=bk_g[:], func=AF.Copy))
                bk_u, ku = lup(TG[0:16, :], GKU[0:16, :], 256, 1024, ["TG", "GKU"])
                op("act", [ku], ["k2"], lambda e: e.activation(out=ew[:, 0:256], in_=bk_u[:, 0:256], func=AF.Sigmoid))
                op("act", ["k2"], ["lsp"], lambda e: e.activation(out=lsp[:], in_=ew[:, 0:256], func=AF.Ln))
                op("dve", ["lsp"], ["lsh"], lambda e: e.tensor_copy(out=lsh[:], in_=lsp[:]))
                op("dve", ["lsp", "lsh"], ["lsl"], lambda e: e.tensor_tensor(out=lsl[:], in0=lsp[:], in1=lsh[:], op=ALU.subtract))

                def cum(mask, hi, lo, n, keys, bank=None, bk=None, col=0):
                    if bank is None:
                        bank, bk = G()
                    op("pe", ["maskb"] + keys, [bk], lambda e: e.matmul(
                        bank[:, col:col + n], lhsT=mask, rhs=hi, start=True, stop=False), inc=False)
                    op("pe", ["maskb"] + keys, [bk], lambda e: e.matmul(
                        bank[:, col:col + n], lhsT=mask, rhs=lo, start=False, stop=True))
                    return bank, bk

                SG = ["sgh", "sgl"]
                bP, kP = cum(mb_iu, sgh[:], sgl[:], 512, SG)
                bX, kX = cum(mb_su, sgh[:], sgl[:], 512, SG)
                bS, kS = cum(mb_sl, sgh[:], sgl[:], 512, SG)
                for p in range(4):
                    op("pe", SG + ["indb"], ["sb7"], lambda e, p=p: e.matmul(
                        sb7[:, 2 * p:2 * p + 2], lhsT=sgh[:, p * 128:(p + 1) * 128], rhs=indb[:, 0:2],
                        start=True, stop=False), inc=False)
                    op("pe", SG + ["indb"], ["sb7"], lambda e, p=p: e.matmul(
                        sb7[:, 2 * p:2 * p + 2], lhsT=sgl[:, p * 128:(p + 1) * 128], rhs=indb[:, 0:2],
                        start=False, stop=True), inc=(p == 3))
                op("act", ["sb7"], ["WC"], lambda e: e.activation(
                    out=WC[:].rearrange("p a b -> p (a b)"), in_=sb7[:, 0:8], func=AF.Exp, scale=CDEC))
                op("pool", ["kz", "CB"], ["sg"], lambda e: e.tensor_tensor(out=t1[:], in0=kz[:], in1=CBkk, op=ALU.mult))
                op("pool", ["sg"], ["kk"], lambda e: e.tensor_tensor(out=kk[:], in0=t1[:], in1=t1[:], op=ALU.mult))
                op("dve", ["kk"], ["ssq"], lambda e: e.tensor_reduce(out=sm[:, 8:16], in_=h3(kk[:], 8), axis=AX.X, op=ALU.add))
                op("dve", ["ssq"], ["ssq"], lambda e: e.tensor_scalar(
                    out=sm[:, 8:16], in0=sm[:, 8:16], scalar1=1e-24, scalar2=None, op0=ALU.max))
                op("act", ["ssq"], ["ssq"], lambda e: e.activation(out=sm[:, 8:16], in_=sm[:, 8:16], func=AF.Ln))
                op("act", ["ssq"], ["rs"], lambda e: e.activation(out=sm[:, 16:24], in_=sm[:, 8:16], func=AF.Exp, scale=-0.5))
                op("dve", ["sg", "rs"], ["kk"], lambda e: e.tensor_tensor(
                    out=h3(kk[:], 8), in0=h3(t1[:], 8), in1=bc(sm[:, 16:24], 64), op=ALU.mult))
                op("dve", ["av", "CB"], ["bv"], lambda e: e.scalar_tensor_tensor(
                    out=bv[:], in0=av[:], scalar=-1.0, in1=CBka, op0=ALU.add, op1=ALU.mult))
                op("dve", ["bv", "kz"], ["k2"], lambda e: e.scalar_tensor_tensor(
                    out=k2[:], in0=bv[:], scalar=1.0, in1=kz[:], op0=ALU.add, op1=ALU.mult))
                op("dve", ["kk", "av"], ["bv"], lambda e: e.tensor_tensor(out=bv[:], in0=kk[:], in1=av[:], op=ALU.mult))
                op("pool", ["rz", "CB"], ["sg"], lambda e: e.tensor_tensor(out=t1[:], in0=rz[:], in1=CBrk, op=ALU.mult))
                op("pool", ["sg", "k2"], ["sg"], lambda e: e.tensor_tensor(out=t1[:], in0=t1[:], in1=k2[:], op=ALU.mult))
                op("dve", ["sg"], ["bs"], lambda e: e.tensor_reduce(out=sm[:, 24:32], in_=h3(t1[:], 8), axis=AX.X, op=ALU.add))
                op("act", [kP], ["E0"], lambda e: e.activation(out=E0[:], in_=bP[:], func=AF.Exp, scale=CDEC))
                op("dve", ["rz", "E0"], ["Rt"], lambda e: e.tensor_tensor(out=Rt[:], in0=rz[:], in1=E0[:], op=ALU.mult))
                op("act", [kX], ["E1"], lambda e: e.activation(out=E1[:], in_=bX[:], func=AF.Exp, scale=CDEC))
                op("dve", ["kk", "E1"], ["At"], lambda e: e.scalar_tensor_tensor(
                    out=At[:], in0=kk[:], scalar=-1.0, in1=E1[:], op0=ALU.mult, op1=ALU.mult))
                op("act", ["At"], ["XA1"], lambda e: e.activation(out=XA[:, :, 1, :], in_=h3(At[:], 8), func=AF.Copy))
                op("act", [kP], ["E0"], lambda e: e.activation(out=E0[:], in_=bP[:], func=AF.Exp, scale=-CDEC))
                op("dve", ["bv", "E0"], ["Bt"], lambda e: e.tensor_tensor(out=Bt[:], in0=bv[:], in1=E0[:], op=ALU.mult))
                op("dve", ["k2", "E0"], ["Kt"], lambda e: e.tensor_tensor(out=Kt[:], in0=k2[:], in1=E0[:], op=ALU.mult))
                op("act", [kS], ["E1"], lambda e: e.activation(out=E1[:], in_=bS[:], func=AF.Exp, scale=CDEC))
                op("dve", ["bv", "E1"], ["Be"], lambda e: e.tensor_tensor(out=Be[:], in0=bv[:], in1=E1[:], op=ALU.mult))
                op("dve", ["k2", "E1"], ["Ke"], lambda e: e.tensor_tensor(out=Ke[:], in0=k2[:], in1=E1[:], op=ALU.mult))
                LS = ["lsh", "lsl"]
                bPg, kPg = cum(mb_iu, lsh[:], lsl[:], 256, LS)
                cum(mb_sl, lsh[:], lsl[:], 256, LS, bPg, kPg, 256)
                op("act", [kPg], ["Eg0"], lambda e: e.activation(out=Eg[:, 0, :], in_=bPg[:, 0:256], func=AF.Exp, scale=1.0 / 16))
                op("act", [kPg], ["Eg1"], lambda e: e.activation(out=Eg[:, 1, :], in_=bPg[:, 0:256], func=AF.Exp, scale=-1.0 / 16))
                op("act", [kPg], ["Eg2"], lambda e: e.activation(out=Eg[:, 2, :], in_=bPg[:, 256:512], func=AF.Exp, scale=1.0 / 16))
                for p in range(2):
                    op("pe", LS + ["indb"], ["sb7"], lambda e, p=p: e.matmul(
                        sb7[:, 16 + 2 * p:18 + 2 * p], lhsT=lsh[:, p * 128:(p + 1) * 128], rhs=indb[:, 0:2],
                        start=True, stop=False), inc=False)
                    op("pe", LS + ["indb"], ["sb7"], lambda e, p=p: e.matmul(
                        sb7[:, 16 + 2 * p:18 + 2 * p], lhsT=lsl[:, p * 128:(p + 1) * 128], rhs=indb[:, 0:2],
                        start=False, stop=True), inc=(p == 1))
                op("act", ["sb7"], ["DEC"], lambda e: e.activation(
                    out=DEC[:].rearrange("p a b -> p (a b)"), in_=sb7[:, 16:20], func=AF.Exp, scale=1.0 / 16))

                CP(4)
                tp4 = tpb[:].rearrange("p (a q) t -> p a q t", a=2)
                for q in range(4):
                    op("pe", ["At", "identb"], ["tpb"], lambda e, q=q: e.transpose(
                        out=tp4[:, 0, q, :], in_=At[:, q * 128:(q + 1) * 128], identity=identb[:]), inc=False)
                for q in range(4):
                    op("pe", ["Rt", "identb"], ["tpb"], lambda e, q=q: e.transpose(
                        out=tp4[:, 1, q, :], in_=Rt[:, q * 128:(q + 1) * 128], identity=identb[:]), inc=(q == 3))
                CP(41)
                op("act", ["tpb"], ["ART"], lambda e: e.activation(out=ART[:, :, 0, :], in_=tp4[:, 0, :, :], func=AF.Copy))
                op("act", ["tpb", "ART"], ["ART"], lambda e: e.activation(out=ART[:, :, 1, :], in_=tp4[:, 1, :, :], func=AF.Copy))
                op("act", ["tpb"], ["RT0"], lambda e: e.activation(out=RT0[:, :, 0:64], in_=tp4[:, 1, :, 0:64], func=AF.Copy))
                op("act", ["tpb"], ["RT1"], lambda e: e.activation(out=RT1[:, :, 64:128], in_=tp4[:, 1, :, 64:128], func=AF.Copy))
                CP(42)
                for q in range(4):
                    op("pe", ["Bt", "identb"], ["tpb"], lambda e, q=q: e.transpose(
                        out=tp4[:, 0, q, :], in_=Bt[:, q * 128:(q + 1) * 128], identity=identb[:]), inc=False)
                for q in range(4):
                    op("pe", ["Kt", "identb"], ["tpb"], lambda e, q=q: e.transpose(
                        out=tp4[:, 1, q, :], in_=Kt[:, q * 128:(q + 1) * 128], identity=identb[:]), inc=(q == 3))
                op("act", ["tpb"], ["BKT"], lambda e: e.activation(
                    out=BKT[:].rearrange("p a q t -> p (a q t)"), in_=tpb[:].rearrange("p k t -> p (k t)"), func=AF.Copy))

                CP(43)
                for p in range(4):
                    if p == 1:
                        CP(46)
                    bank, bk = G()
                    for e_ in range(2):
                        rows = slice(64 * e_, 64 * e_ + 64)
                        op("pe", ["BKT", "ART"], [bk], lambda e, rows=rows, e_=e_, p=p, bank=bank: e.matmul(
                            bank[:, e_ * 256:(e_ + 1) * 256], lhsT=BKT[rows, 0, p, :],
                            rhs=ART[rows, p, :, :].rearrange("p a t -> p (a t)"), start=True, stop=True), inc=(e_ == 1), rg=rows.start)
                    if p == 0:
                        CP(44)
                    op("dve", [bk, "maskt"], ["QL%d" % p], lambda e, p=p, bank=bank: e.tensor_tensor(
                        out=QL[:, 2 * p:2 * p + 2, :, :].rearrange("p h a t -> p h (a t)"),
                        in0=bank[:].rearrange("p (h c) -> p h c", h=2), in1=MK1, op=ALU.mult))
                    bank, bk = G()
                    for e_ in range(2):
                        rows = slice(64 * e_, 64 * e_ + 64)
                        op("pe", ["BKT", "ART"], [bk], lambda e, rows=rows, e_=e_, p=p, bank=bank: e.matmul(
                            bank[:, e_ * 256:(e_ + 1) * 256], lhsT=BKT[rows, 1, p, :],
                            rhs=ART[rows, p, :, :].rearrange("p a t -> p (a t)"), start=True, stop=True), inc=(e_ == 1), rg=rows.start)
                    op("dve", [bk, "maskt"], ["AL%d" % p], lambda e, p=p, bank=bank: e.tensor_tensor(
                        out=AL[:, 2 * p:2 * p + 2, :, :].rearrange("p h a t -> p h (a t)"),
                        in0=bank[:].rearrange("p (h c) -> p h c", h=2), in1=MK1, op=ALU.mult))
                QLk = ["QL%d" % p for p in range(4)]
                ALk = ["AL%d" % p for p in range(4)]

                CP(5)
                for hg in range(2):
                    hs = slice(4 * hg, 4 * hg + 4)
                    bank, bk = G()
                    for j in range(4):
                        h = 4 * hg + j
                        p, rows = h // 2, slice(64 * (h % 2), 64 * (h % 2) + 64)
                        op("pe", ["BKT", "ART"], [bk], lambda e, rows=rows, j=j, p=p, bank=bank: e.matmul(
                            bank[:, j * 128:(j + 1) * 128], lhsT=ART[rows, p, 0, :], rhs=BKT[rows, 0, p, :],
                            start=True, stop=True), inc=(j == 3), rg=rows.start)
                    op("dve", [bk, "maskt"], ["Qg0"], lambda e, bank=bank: e.tensor_tensor(
                        out=Qg[0][:], in0=bank[:].rearrange("p (h c) -> p h c", h=4), in1=MK3, op=ALU.mult))
                    op("dve", QLk, ["QTg0"], lambda e: e.tensor_copy(out=QTg[0][:], in_=QL[:, hs, 0, :]))
                    op("dve", QLk + ["identb"], ["PTg0"], lambda e: e.tensor_tensor(
                        out=PTg[0][:], in0=QL[:, hs, 0, :],
                        in1=identb[:].unsqueeze(1).to_broadcast([128, 4, 128]), op=ALU.add))
                    for lv in range(1, 6):
                        c, n_ = (lv - 1) % 2, lv % 2
                        bank, bk = G()
                        for j in range(4):
                            op("pe", ["Qg%d" % c, "QTg%d" % c], [bk], lambda e, j=j, bank=bank: e.matmul(
                                bank[:, j * 128:(j + 1) * 128], lhsT=QTg[c][:, j, :], rhs=Qg[c][:, j, :],
                                start=True, stop=True), inc=(j == 3))
                        op("act", [bk], ["Qg%d" % n_], lambda e, bank=bank: e.activation(
                            out=Qg[n_][:].rearrange("p h t -> p (h t)"), in_=bank[:], func=AF.Copy))
                        if lv < 5:
                            bank, bk = G()
                            for j in range(4):
                                op("pe", ["Qg%d" % c, "QTg%d" % c], [bk], lambda e, j=j, bank=bank: e.matmul(
                                    bank[:, j * 128:(j + 1) * 128], lhsT=Qg[c][:, j, :], rhs=QTg[c][:, j, :],
                                    start=True, stop=True), inc=(j == 3))
                            op("dve", [bk], ["QTg%d" % n_], lambda e, bank=bank: e.tensor_copy(
                                out=QTg[n_][:].rearrange("p h t -> p (h t)"), in_=bank[:]))
                        bank, bk = G()
                        for j in range(4):
                            op("pe", ["identb", "PTg%d" % c], [bk], lambda e, j=j, bank=bank: e.matmul(
                                bank[:, j * 128:(j + 1) * 128], lhsT=identb[:], rhs=PTg[c][:, j, :],
                                start=True, stop=False), inc=False)
                            op("pe", ["Qg%d" % n_, "PTg%d" % c], [bk], lambda e, j=j, bank=bank: e.matmul(
                                bank[:, j * 128:(j + 1) * 128], lhsT=Qg[n_][:, j, :], rhs=PTg[c][:, j, :],
                                start=False, stop=True), inc=(j == 3))
                        if lv < 5:
                            dst, dk_ = PTg[n_][:].rearrange("p h t -> p (h t)"), "PTg%d" % n_
                        else:
                            dst, dk_ = PTf[:, hs, :].rearrange("p h t -> p (h t)"), "PTf%d" % hg
                        if lv % 2:
                            op("act", [bk], [dk_], lambda e, bank=bank, dst=dst: e.activation(out=dst, in_=bank[:], func=AF.Copy))
                        else:
                            op("dve", [bk], [dk_], lambda e, bank=bank, dst=dst: e.tensor_copy(out=dst, in_=bank[:]))
                PTk = ["PTf0", "PTf1"]

                CP(6)
                bank, bk = G()
                for h in range(8):
                    op("pe", ALk + ["vb"], [bk], lambda e, h=h, bank=bank: e.matmul(
                        bank[:, h * 64:(h + 1) * 64], lhsT=AL[:, h, 0, :], rhs=vb[:, h * 64:(h + 1) * 64],
                        start=True, stop=True), inc=(h == 7))
                CP(66)
                op("dve", [bk], ["XA0"], lambda e, bank=bank: e.tensor_copy(
                    out=XA[:, :, 0, :], in_=h3(bank[:], 8)))
                CP(67)
                for hg in range(2):
                    bank, bk = G()
                    for j in range(4):
                        h = 4 * hg + j
                        op("pe", PTk + ["XA0", "XA1"], [bk], lambda e, j=j, h=h, bank=bank: e.matmul(
                            bank[:, j * 128:(j + 1) * 128], lhsT=PTf[:, h, :],
                            rhs=XA[:, h, :, :].rearrange("p a d -> p (a d)"), start=True, stop=True), inc=(j == 3))
                    bv4 = bank[:].rearrange("p (h a d) -> p h a d", h=4, a=2)
                    op("dve", [bk], ["UL32_%d" % hg], lambda e, hg=hg, bv4=bv4: e.tensor_copy(
                        out=UL32[:, 4 * hg:4 * hg + 4, :], in_=bv4[:, :, 0, :]))
                    op("dve", [bk], ["Ahb_%d" % hg], lambda e, hg=hg, bv4=bv4: e.tensor_copy(
                        out=Ahb[:, 4 * hg:4 * hg + 4, :], in_=bv4[:, :, 1, :]))
                CP(68)
                for q in range(4):
                    op("pe", ["Ahb_0", "Ahb_1", "identb"], ["tpb"], lambda e, q=q: e.transpose(
                        out=tp4[:, 0, q, :], in_=Ahb[:].rearrange("p h d -> p (h d)")[:, q * 128:(q + 1) * 128], identity=identb[:]), inc=(q == 3))
                op("act", ["tpb"], ["AT0"], lambda e: e.activation(out=AT0[:, :, 0:64], in_=tp4[:, 0, :, 0:64], func=AF.Copy))
                op("act", ["tpb"], ["AT1"], lambda e: e.activation(out=AT1[:, :, 64:128], in_=tp4[:, 0, :, 64:128], func=AF.Copy))

                CP(61)
                for h in range(8):
                    p, rows = h // 2, slice(64 * (h % 2), 64 * (h % 2) + 64)
                    op("pe", ["AT0", "Mb"], ["sb7"], lambda e, h=h, p=p, rows=rows: e.matmul(
                        sb7[:, h * 64:(h + 1) * 64], lhsT=AT0[rows, p, :], rhs=Mb[rows, p, 0, :], start=True, stop=False), inc=False, rg=rows.start)
                    op("pe", ["AT1", "Mb"], ["sb7"], lambda e, h=h, p=p, rows=rows: e.matmul(
                        sb7[:, h * 64:(h + 1) * 64], lhsT=AT1[rows, p, :], rhs=Mb[rows, p, 1, :], start=False, stop=True), inc=(h == 7), rg=rows.start)
                op("dve", ["sb7", "UL32_0", "UL32_1"], ["Ub"], lambda e: e.tensor_tensor(
                    out=Ub[:], in0=h3(sb7[:], 8), in1=UL32[:], op=ALU.add))
                CP(62)
                bY, kY = G()
                for h in range(8):
                    p, rows = h // 2, slice(64 * (h % 2), 64 * (h % 2) + 64)
                    o_ = bY[:, h * 64:(h + 1) * 64]
                    op("pe", ["RT0", "Mb"], [kY], lambda e, o_=o_, p=p, rows=rows: e.matmul(
                        o_, lhsT=RT0[rows, p, :], rhs=Mb[rows, p, 0, :], start=True, stop=False), inc=False, rg=rows.start)
                    op("pe", ["RT1", "Mb"], [kY], lambda e, o_=o_, p=p, rows=rows: e.matmul(
                        o_, lhsT=RT1[rows, p, :], rhs=Mb[rows, p, 1, :], start=False, stop=False), inc=False, rg=rows.start)
                    op("pe", QLk + ["Ub"], [kY], lambda e, o_=o_, h=h: e.matmul(
                        o_, lhsT=QL[:, h, 1, :], rhs=Ub[:, h, :], start=False, stop=False), inc=False)
                    op("pe", ALk + ["vb"], [kY], lambda e, o_=o_, h=h: e.matmul(
                        o_, lhsT=AL[:, h, 1, :], rhs=vb[:, h * 64:(h + 1) * 64], start=False, stop=True), inc=(h == 7))
                CP(63)
                dMk = []
                dMb = []
                for half in range(2):
                    bank, bk = (sb7, "sb7") if half == 0 else G()
                    dMk.append(bk)
                    dMb.append(bank)
                    for pp in range(2):
                        p = 2 * half + pp
                        for s_ in range(2):
                            rs_ = slice(64 * s_, 64 * s_ + 64)
                            o_ = bank[:, (pp * 2 + s_) * 128:(pp * 2 + s_ + 1) * 128]
                            op("pe", ["Be", "Ub"], [bk], lambda e, o_=o_, p=p, rs_=rs_: e.matmul(
                                o_, lhsT=Be[rs_, p * 128:(p + 1) * 128],
                                rhs=Ub[rs_, 2 * p:2 * p + 2, :].rearrange("p h d -> p (h d)"), start=True, stop=False), inc=False, rg=rs_.start)
                            op("pe", ["Ke", "vb"], [bk], lambda e, o_=o_, p=p, rs_=rs_: e.matmul(
                                o_, lhsT=Ke[rs_, p * 128:(p + 1) * 128], rhs=vb[rs_, p * 128:(p + 1) * 128],
                                start=False, stop=True), inc=(pp == 1 and s_ == 1), rg=rs_.start)
                CP(64)
                op("dve", ["M32", "WC"], ["tmp2k"], lambda e: e.tensor_tensor(
                    out=mtmp.rearrange("p a b d -> p (a b) d"), in0=M32[:].rearrange("p a b d -> p (a b) d"),
                    in1=bc(WC[:].rearrange("p a b -> p (a b)"), 64), op=ALU.mult))
                M32v = M32[:].rearrange("p a b d -> p (a b) d")
                mtv = mtmp.rearrange("p a b d -> p (a b) d")
                for half in range(2):
                    dv = dMb[half][:].rearrange("p (c d) -> p c d", c=4)
                    hsl = slice(4 * half, 4 * half + 4)
                    op("dve", ["tmp2k", dMk[half]], ["M32"], lambda e, hsl=hsl, dv=dv: e.tensor_tensor(
                        out=M32v[0:64, hsl, :], in0=mtv[0:64, hsl, :], in1=dv[0:64, :, 0:64], op=ALU.add))
                    op("dve", ["tmp2k", dMk[half]], ["M32"], lambda e, hsl=hsl, dv=dv: e.tensor_tensor(
                        out=M32v[64:128, hsl, :], in0=mtv[64:128, hsl, :], in1=dv[64:128, :, 64:128], op=ALU.add))
                op("act", ["M32"], ["Mb"], lambda e: e.activation(
                    out=Mb[:].rearrange("p a b d -> p (a b d)"), in_=M32[:].rearrange("p a b d -> p (a b d)"), func=AF.Copy))

                CP(65)
                op("dve", [kY], ["s1"], lambda e: e.tensor_reduce(out=sm[:, 32:40], in_=h3(bY[:], 8), axis=AX.X, op=ALU.add))
                op("act", [kY], ["E0"], lambda e: e.activation(out=ysq[:], in_=bY[:], func=AF.Square))
                op("dve", ["E0"], ["s2"], lambda e: e.tensor_reduce(out=sm[:, 40:48], in_=h3(ysq[:], 8), axis=AX.X, op=ALU.add))
                op("dve", ["s1"], ["s1"], lambda e: e.tensor_scalar(
                    out=sm[:, 32:40], in0=sm[:, 32:40], scalar1=1.0 / 64, scalar2=None, op0=ALU.mult))
                op("dve", ["s1"], ["msq"], lambda e: e.tensor_tensor(out=sm[:, 48:56], in0=sm[:, 32:40], in1=sm[:, 32:40], op=ALU.mult))
                op("dve", ["s2", "msq"], ["s2"], lambda e: e.scalar_tensor_tensor(
                    out=sm[:, 40:48], in0=sm[:, 40:48], scalar=1.0 / 64, in1=sm[:, 48:56], op0=ALU.mult, op1=ALU.subtract))
                op("act", ["s2"], ["s2"], lambda e: e.activation(out=sm[:, 40:48], in_=sm[:, 40:48], func=AF.Ln, bias=epsc[:, 1:2]))
                op("act", ["s2"], ["s2"], lambda e: e.activation(out=sm[:, 40:48], in_=sm[:, 40:48], func=AF.Exp, scale=-0.5))
                op("dve", [kY, "s1"], ["E1"], lambda e: e.tensor_tensor(
                    out=h3(yn[:], 8), in0=h3(bY[:], 8), in1=bc(sm[:, 32:40], 64), op=ALU.subtract))
                op("dve", ["E1", "s2"], ["E1"], lambda e: e.tensor_tensor(
                    out=h3(yn[:], 8), in0=h3(yn[:], 8), in1=bc(sm[:, 40:48], 64), op=ALU.mult))
                op("pool", ["E1", "CB"], ["E1"], lambda e: e.tensor_tensor(out=yn[:], in0=yn[:], in1=CBlw, op=ALU.mult))
                op("pool", ["E1", "CB"], ["E1"], lambda e: e.tensor_tensor(out=yn[:], in0=yn[:], in1=CBlb, op=ALU.add))
                op("dve", ["vb", "bs", "sg"], ["sg"], lambda e: e.tensor_tensor(
                    out=h3(t1[:], 8), in0=h3(vb[:], 8), in1=bc(sm[:, 24:32], 64), op=ALU.mult))
                op("pool", ["E1", "sg"], ["E1"], lambda e: e.tensor_tensor(out=yn[:], in0=yn[:], in1=t1[:], op=ALU.add))
                op("dve", ["E1", "gz"], ["MIXr"], lambda e: e.tensor_tensor(out=MIX[:, 0:512], in0=yn[:], in1=gz[:], op=ALU.mult))

                CP(7)
                op("dve", ["qkz", "Eg0"], ["QI"], lambda e: e.scalar_tensor_tensor(
                    out=QI[:], in0=qkz[:, 0:256], scalar=0.125, in1=Eg[:, 0, :], op0=ALU.mult, op1=ALU.mult))
                op("dve", ["qkz", "Eg1"], ["KI"], lambda e: e.tensor_tensor(out=KI[:], in0=qkz[:, 256:512], in1=Eg[:, 1, :], op=ALU.mult))
                op("dve", ["qkz", "Eg2"], ["KE"], lambda e: e.tensor_tensor(out=KE[:], in0=qkz[:, 256:512], in1=Eg[:, 2, :], op=ALU.mult))
                for q in range(2):
                    op("pe", ["QI", "identb"], ["tpb"], lambda e, q=q: e.transpose(
                        out=tp4[:, 1, q, :], in_=QI[:, q * 128:(q + 1) * 128], identity=identb[:]), inc=False)
                for q in range(2):
                    op("pe", ["KI", "identb"], ["tpb"], lambda e, q=q: e.transpose(
                        out=tp4[:, 1, 2 + q, :], in_=KI[:, q * 128:(q + 1) * 128], identity=identb[:]), inc=(q == 1))
                op("act", ["tpb"], ["GT"], lambda e: e.activation(out=GT[:], in_=tp4[:, 1, :, :], func=AF.Copy))
                op("act", ["tpb"], ["QG0"], lambda e: e.activation(out=QG0[:, :, 0:64], in_=tp4[:, 1, 0:2, 0:64], func=AF.Copy))
                op("act", ["tpb"], ["QG1"], lambda e: e.activation(out=QG1[:, :, 64:128], in_=tp4[:, 1, 0:2, 64:128], func=AF.Copy))
                bank, bk = G()
                for h in range(4):
                    p, rows = h // 2, slice(64 * (h % 2), 64 * (h % 2) + 64)
                    op("pe", ["GT"], [bk], lambda e, h=h, p=p, rows=rows, bank=bank: e.matmul(
                        bank[:, h * 128:(h + 1) * 128], lhsT=GT[rows, 2 + p, :], rhs=GT[rows, p, :], start=True, stop=True),
                        inc=(h == 3), rg=rows.start)
                op("dve", [bk, "maskt"], ["attT"], lambda e, bank=bank: e.tensor_tensor(
                    out=attT[:], in0=bank[:].rearrange("p (h c) -> p h c", h=4), in1=MKiu, op=ALU.mult))
                bO, kO = G()
                for h in range(4):
                    p, rows = h // 2, slice(64 * (h % 2), 64 * (h % 2) + 64)
                    o_ = bO[:, h * 128:(h + 1) * 128]
                    op("pe", ["attT", "gvb"], [kO], lambda e, o_=o_, h=h: e.matmul(
                        o_, lhsT=attT[:, h, :], rhs=gvb[:, h * 128:(h + 1) * 128], start=True, stop=False), inc=False)
                    op("pe", ["QG0", "Sb"], [kO], lambda e, o_=o_, p=p, rows=rows: e.matmul(
                        o_, lhsT=QG0[rows, p, :], rhs=Sb[rows, p, 0, :], start=False, stop=False), inc=False, rg=rows.start)
                    op("pe", ["QG1", "Sb"], [kO], lambda e, o_=o_, p=p, rows=rows: e.matmul(
                        o_, lhsT=QG1[rows, p, :], rhs=Sb[rows, p, 1, :], start=False, stop=True), inc=(h == 3), rg=rows.start)
                dSk = []
                dSb = []
                for p in range(2):
                    bank, bk = (sb7, "sb7") if p == 0 else G()
                    dSk.append(bk)
                    dSb.append(bank)
                    for s_ in range(2):
                        rs_ = slice(64 * s_, 64 * s_ + 64)
                        op("pe", ["KE", "gvb"], [bk], lambda e, p=p, s_=s_, rs_=rs_, bank=bank: e.matmul(
                            bank[:, s_ * 256:(s_ + 1) * 256], lhsT=KE[rs_, p * 128:(p + 1) * 128],
                            rhs=gvb[rs_, p * 256:(p + 1) * 256], start=True, stop=True), inc=(s_ == 1), rg=rs_.start)
                op("dve", ["S32", "DEC"], ["tmp2k"], lambda e: e.tensor_tensor(
                    out=stmp.rearrange("p a b d -> p (a b) d"), in0=S32[:].rearrange("p a b d -> p (a b) d"),
                    in1=bc(DEC[:].rearrange("p a b -> p (a b)"), 128), op=ALU.mult))
                for p in range(2):
                    dv = dSb[p][:].rearrange("p (b d) -> p b d", b=2)
                    op("dve", ["tmp2k", dSk[p]], ["S32"], lambda e, p=p, dv=dv: e.tensor_tensor(
                        out=S32[0:64, p, :, :], in0=stmp[0:64, p, :, :], in1=dv[0:64, :, 0:128], op=ALU.add))
                    op("dve", ["tmp2k", dSk[p]], ["S32"], lambda e, p=p, dv=dv: e.tensor_tensor(
                        out=S32[64:128, p, :, :], in0=stmp[64:128, p, :, :], in1=dv[64:128, :, 128:256], op=ALU.add))
                op("act", ["S32"], ["Sb"], lambda e: e.activation(
                    out=Sb[:].rearrange("p a b d -> p (a b d)"), in_=S32[:].rearrange("p a b d -> p (a b d)"), func=AF.Copy))
                op("act", [kO], ["E0"], lambda e: e.activation(out=ysq[:], in_=bO[:], func=AF.Square))
                op("dve", ["E0"], ["ss4"], lambda e: e.tensor_reduce(out=sm[:, 56:60], in_=h3(ysq[:], 4), axis=AX.X, op=ALU.add))
                op("act", ["ss4"], ["ss4"], lambda e: e.activation(
                    out=sm[:, 56:60], in_=sm[:, 56:60], func=AF.Ln, scale=1.0 / 128, bias=epsc[:, 2:3]))
                op("act", ["ss4"], ["ors"], lambda e: e.activation(out=sm[:, 60:64], in_=sm[:, 56:60], func=AF.Exp, scale=-0.5))
                op("dve", [kO, "ors"], ["E1"], lambda e: e.tensor_tensor(
                    out=h3(yn[:], 4), in0=h3(bO[:], 4), in1=bc(sm[:, 60:64], 128), op=ALU.mult))
                op("pool", ["E1", "CB"], ["E1"], lambda e: e.tensor_tensor(out=h3(yn[:], 4), in0=h3(yn[:], 4), in1=CBgn, op=ALU.mult))
                op("dve", ["E1", "osl"], ["MIXg"], lambda e: e.tensor_tensor(out=MIX[:, 512:1024], in0=yn[:], in1=osl[:], op=ALU.mult))
                dma("sp", mixscr[0, i * 64:(i + 1) * 64, :], MIX[0:64, :], ["MIXr", "MIXg"], ["mscr"], "MIXa")
                dma("sp", mixscr[1, i * 64:(i + 1) * 64, :], MIX[64:128, :], ["MIXr", "MIXg"], ["mscr"], "MIXb")
            B.barrier()
        B.es = es0
        CP(8)

        with ExitStack() as es2:
            B.es = es2
            gc2 = B.sb("gc2", [128, 24], F32)
            FG = B.sb("FG", [128, D], F32)
            Wo = B.sb("Wo", [128, 8, D], BF16)
            Wg = B.sb("Wg", [128, 8, DFF], BF16)
            Wu = B.sb("Wu", [128, 8, DFF], BF16)
            Wd = B.sb("Wd", [128, NFF, D], BF16)
            dma("sp", gc2[:], gcols[:, :], [], ["gc2"], "c2")
            dma("sp", FG[:], fg_row[0:1, :].partition_broadcast(128), [], ["FG"], "c2")
            B.group_done("c2", ["gc2", "FG"])
            with ExitStack() as es_s:
                B.es = es_s
                st = [B.sb("st%d" % i, [128, DFF], F32) for i in range(2)]
                cnt = [0]

                def stage(src, rows, ncol, dst, scale_col):
                    i_ = cnt[0] % 2
                    cnt[0] += 1
                    key = "st%d" % i_
                    dma("sp", st[i_][:, 0:ncol], src[rows, :], [], [key], key)
                    eng = "act" if cnt[0] % 2 else "dve"
                    if scale_col is None:
                        if eng == "act":
                            op("act", [key], ["W2"], lambda e: e.activation(out=dst, in_=st[i_][:, 0:ncol], func=AF.Copy))
                        else:
                            op("dve", [key], ["W2"], lambda e: e.tensor_copy(out=dst, in_=st[i_][:, 0:ncol]))
                    else:
                        if eng == "act":
                            op("act", [key, "gc2"], ["W2"], lambda e: e.activation(
                                out=dst, in_=st[i_][:, 0:ncol], func=AF.Copy, scale=scale_col))
                        else:
                            op("dve", [key, "gc2"], ["W2"], lambda e: e.tensor_scalar(
                                out=dst, in0=st[i_][:, 0:ncol], scalar1=scale_col, scalar2=None, op0=ALU.mult))

                for k in range(8):
                    stage(w_out, slice(k * 128, (k + 1) * 128), D, Wo[:, k, :], None)
                for k in range(8):
                    stage(ffn_gate, slice(k * 128, (k + 1) * 128), DFF, Wg[:, k, :], gc2[:, 8 + k:9 + k])
                    stage(ffn_up, slice(k * 128, (k + 1) * 128), DFF, Wu[:, k, :], gc2[:, 8 + k:9 + k])
                for j in range(NFF):
                    stage(ffn_down, slice(j * 128, (j + 1) * 128), D, Wd[:, j, :], None)
                B.barrier()
            B.es = es2
            CP(9)

            xh = B.sb("xh", [128, NS2, D], F32)
            mxl = B.sb("mxl", [128, NS2, D], BF16)
            mT = B.sb("mT", [128, 8, TOK2], BF16)
            n2 = B.sb("n2", [128, D], BF16)
            n2T = B.sb("n2T", [128, 8, TOK2], BF16)
            actT = B.sb("actT", [128, NFF, TOK2], BF16)
            sil = B.sb("sil", [128, TOK2], F32)
            ob = B.sb("ob", [128, NS2, D], F32)
            junk2 = B.sb("junk2", [128, D], BF16)
            sm2 = B.sb("sm2", [128, 16], F32)

            def rstd_of(src_ap, keys, col, tag):
                op("act", keys, ["junk2", tag + "ss"], lambda e: e.activation(
                    out=junk2[:], in_=src_ap, func=AF.Square, accum_out=sm2[:, col:col + 1]))
                op("act", [tag + "ss"], [tag + "ms"], lambda e: e.activation(
                    out=sm2[:, col + 1:col + 2], in_=sm2[:, col:col + 1], func=AF.Ln, scale=1.0 / D, bias=epsc[:, 0:1]))
                op("act", [tag + "ms"], [tag + "rs"], lambda e: e.activation(
                    out=sm2[:, col + 2:col + 3], in_=sm2[:, col + 1:col + 2], func=AF.Exp, scale=-0.5))
                return sm2[:, col + 2:col + 3], tag + "rs"

            for it in range(NT2):
                b_ = (it * TOK2) // T
                t0 = (it * TOK2) % T
                for u in range(NS2):
                    dma("sp", xh[:, u, :], x[b_, t0 + u * 128:t0 + (u + 1) * 128, :], [], ["xh%d" % u], "xh%d" % u)
                    dma("sp", mxl[:, u, :], mixscr[b_, t0 + u * 128:t0 + (u + 1) * 128, :], ["mscr"], ["mxl%d" % u], "mxl%d" % u)
                for u in range(NS2):
                    for k in range(8):
                        op("pe", ["mxl%d" % u, "identb"], ["tpb"], lambda e, k=k, u=u: e.transpose(
                            out=tpb[:, k, :], in_=mxl[:, u, k * 128:(k + 1) * 128], identity=identb[:]), inc=(k == 7))
                    op("act", ["tpb"], ["mT%d" % u], lambda e, u=u: e.activation(
                        out=mT[:, :, u * 128:(u + 1) * 128], in_=tpb[:], func=AF.Copy))
                for u in range(NS2):
                    for hf in range(2):
                        bank, bk = zb[hf], "zb%d" % hf
                        for k in range(8):
                            op("pe", ["mT%d" % u, "W2"], [bk], lambda e, k=k, u=u, hf=hf, bank=bank: e.matmul(
                                bank[:], lhsT=mT[:, k, u * 128:(u + 1) * 128], rhs=Wo[:, k, hf * 512:(hf + 1) * 512],
                                start=(k == 0), stop=(k == 7)), inc=(k == 7))
                        op("dve", [bk, "xh%d" % u], ["h%d_%d" % (u, hf)], lambda e, u=u, hf=hf, bank=bank: e.tensor_tensor(
                            out=xh[:, u, hf * 512:(hf + 1) * 512], in0=xh[:, u, hf * 512:(hf + 1) * 512], in1=bank[:], op=ALU.add))
                    hk = ["h%d_0" % u, "h%d_1" % u]
                    rs_ap, rk = rstd_of(xh[:, u, :], hk, 0, "a")
                    op("act", hk + [rk], ["n2"], lambda e, u=u, rs_ap=rs_ap: e.activation(
                        out=n2[:], in_=xh[:, u, :], func=AF.Copy, scale=rs_ap))
                    for k in range(8):
                        op("pe", ["n2", "identb"], ["tpb"], lambda e, k=k: e.transpose(
                            out=tpb[:, k, :], in_=n2[:, k * 128:(k + 1) * 128], identity=identb[:]), inc=(k == 7))
                    op("act", ["tpb"], ["n2T%d" % u], lambda e, u=u: e.activation(out=n2T[:, :, u * 128:(u + 1) * 128], in_=tpb[:], func=AF.Copy))
                nk = ["n2T%d" % u for u in range(NS2)]
                for j in range(NFF):
                    bg, kg_ = gb[(2 * j) % 3], "gb%d" % ((2 * j) % 3)
                    bu, ku_ = gb[(2 * j + 1) % 3], "gb%d" % ((2 * j + 1) % 3)
                    for k in range(8):
                        op("pe", nk + ["W2"], [kg_], lambda e, k=k, j=j, bg=bg: e.matmul(
                            bg[:, 0:TOK2], lhsT=Wg[:, k, j * 128:(j + 1) * 128], rhs=n2T[:, k, :],
                            start=(k == 0), stop=(k == 7)), inc=(k == 7))
                    for k in range(8):
                        op("pe", nk + ["W2"], [ku_], lambda e, k=k, j=j, bu=bu: e.matmul(
                            bu[:, 0:TOK2], lhsT=Wu[:, k, j * 128:(j + 1) * 128], rhs=n2T[:, k, :],
                            start=(k == 0), stop=(k == 7)), inc=(k == 7))
                    op("act", [kg_], ["sil"], lambda e, bg=bg: e.activation(out=sil[:], in_=bg[:, 0:TOK2], func=AF.Silu))
                    op("dve", ["sil", ku_], ["actT%d" % j], lambda e, j=j, bu=bu: e.tensor_tensor(
                        out=actT[:, j, :], in0=sil[:], in1=bu[:, 0:TOK2], op=ALU.mult))
                ak = ["actT%d" % j for j in range(NFF)]
                for u in range(NS2):
                    for hf in range(2):
                        bank, bk = zb[hf], "zb%d" % hf
                        for j in range(NFF):
                            op("pe", ak + ["W2"], [bk], lambda e, j=j, u=u, hf=hf, bank=bank: e.matmul(
                                bank[:], lhsT=actT[:, j, u * 128:(u + 1) * 128], rhs=Wd[:, j, hf * 512:(hf + 1) * 512],
                                start=(j == 0), stop=(j == NFF - 1)), inc=(j == NFF - 1))
                        op("dve", [bk, "h%d_%d" % (u, hf)], ["g%d_%d" % (u, hf)], lambda e, u=u, hf=hf, bank=bank: e.tensor_tensor(
                            out=xh[:, u, hf * 512:(hf + 1) * 512], in0=xh[:, u, hf * 512:(hf + 1) * 512], in1=bank[:], op=ALU.add))
                    gk_ = ["g%d_0" % u, "g%d_1" % u]
                    rs_ap, rk = rstd_of(xh[:, u, :], gk_, 4, "b")
                    op("dve", gk_ + [rk, "FG"], ["ob%d" % u], lambda e, u=u, rs_ap=rs_ap: e.scalar_tensor_tensor(
                        out=ob[:, u, :], in0=xh[:, u, :], scalar=rs_ap, in1=FG[:], op0=ALU.mult, op1=ALU.mult))
                    dma("sp", out[b_, t0 + u * 128:t0 + (u + 1) * 128, :], ob[:, u, :], ["ob%d" % u], ["out"], "ob%d" % u)
            B.barrier()
        B.es = es0


def _consts():
    s = np.arange(128)
    same = (s[:, None] // 64) == (s[None, :] // 64)
    su = (same & (s[:, None] < s[None, :])).astype(np.float32)
    iu = (same & (s[:, None] <= s[None, :])).astype(np.float32)
    sl = (same & (s[:, None] > s[None, :])).astype(np.float32)
    sh = (same & (s[:, None] == s[None, :] - 1)).astype(np.float32)
    carry = np.zeros((128, 128), np.float32)
    carry[63, 0] = 1.0
    carry[127, 64] = 1.0
    cmask = np.concatenate([su, iu, sl, sh, carry], axis=1)
    cind = np.stack([(s < 64), (s >= 64)], axis=1).astype(np.float32)
    return np.eye(128, dtype=np.float32), np.ascontiguousarray(cmask), np.ascontiguousarray(cind)


def make_in_maps(inputs, T, n_cores):
    f = lambda a: np.ascontiguousarray(np.asarray(a, dtype=np.float32))
    ident, cmask, cind = _consts()
    g1 = f(inputs["rms1_g"])[0].reshape(8, 128).T
    g2 = f(inputs["rms2_g"])[0].reshape(8, 128).T
    gcols = np.ascontiguousarray(np.concatenate([g1, g2, g2], axis=1))
    bias_row = np.concatenate([f(inputs["decay_base"])[0], f(inputs["iclr_base"])[0], f(inputs["gk_bias"])[0]])[None, :]
    cb_row = np.concatenate([f(inputs["k_k"])[0], f(inputs["k_a"])[0], f(inputs["r_k"])[0].reshape(-1),
                             f(inputs["lnx_w"])[0], f(inputs["lnx_b"])[0],
                             f(inputs["gla_norm_g"])[0]])[None, :]
    shared = {
        "w_in": f(inputs["w_in"])[0], "w_out": f(inputs["w_out"])[0],
        "ffn_gate": f(inputs["ffn_gate"])[0], "ffn_up": f(inputs["ffn_up"])[0], "ffn_down": f(inputs["ffn_down"])[0],
        "mu_row": f(inputs["mu_shift"]), "gcols": gcols,
        "decay_up": f(inputs["decay_up"])[0], "iclr_up": f(inputs["iclr_up"])[0],
        "gate_up": f(inputs["gate_up"])[0], "gk_up": f(inputs["gk_up"])[0],
        "bias_row": np.ascontiguousarray(bias_row), "cb_row": np.ascontiguousarray(cb_row),
        "fg_row": f(inputs["final_g"])[None, :],
        "cident": ident, "cmask": cmask, "cind": cind,
    }
    xs = f(inputs["x"])
    maps = []
    for c in range(n_cores):
        m = dict(shared)
        m["x"] = np.ascontiguousarray(xs[2 * c:2 * c + 2, :T])
        maps.append(m)
    return maps


def kernel(**inputs):
    T = 2048
    n = 8
    nc = build_program(T)
    in_maps = make_in_maps(inputs, T, n)
    res = run_bass_kernel_spmd(nc, in_maps, core_ids=list(range(n)))
    return np.concatenate([np.asarray(r["out"], dtype=np.float32) for r in res.results], axis=0)
```
